# Optimizing a Trainium2 kernel written in Bass

```python
import jax, jax.numpy as jnp
from jax import lax
import numpy as np

D_MODEL = 1024
BATCH = 4
SEQ = 8192
DEPTH = 1

D_CONV = D_MODEL
CONV_GROUPS = 8
CONV_WIDTH = 3
D_RNN = (5 * D_MODEL) // 4
RNN_BLOCKS = 10
RNN_BLOCK = D_RNN // RNN_BLOCKS
RNN_CONV_WIDTH = 4
LRU_C = 8.0
W_IN_COLS = 3 * D_CONV + 2 * D_RNN + 2 * D_MODEL
SPLITS = (D_CONV, 2 * D_CONV, 3 * D_CONV, 3 * D_CONV + D_RNN,
          3 * D_CONV + 2 * D_RNN, 3 * D_CONV + 2 * D_RNN + D_MODEL)
PEER_HEADS = 8
PEER_NKEYS = 128
PEER_NEXPERTS = PEER_NKEYS * PEER_NKEYS
PEER_QDIM = 256
PEER_HALF = PEER_QDIM // 2
PEER_TOPK = 16
PEER_CHUNK = 128
EPS = 1e-6

kernel_name = "hybrid_conv_rglru_peer"


def rms_norm(x, g):
    xf = x.astype(jnp.float32)
    y = xf * lax.rsqrt(jnp.mean(xf * xf, axis=-1, keepdims=True) + EPS)
    return (y * g.astype(jnp.float32)).astype(x.dtype)


def causal_depthwise_conv(x, w):
    k = w.shape[0]
    s = x.shape[1]
    xp = jnp.pad(x, ((0, 0), (k - 1, 0), (0, 0)))
    out = xp[:, 0:s] * w[0]
    for j in range(1, k):
        out = out + xp[:, j:j + s] * w[j]
    return out


def block_diag_linear(x, w, b):
    bsz, s, _ = x.shape
    xb = x.reshape(bsz, s, RNN_BLOCKS, RNN_BLOCK)
    return jnp.einsum('bsni,nij->bsnj', xb, w).reshape(bsz, s, D_RNN) + b


def rg_lru(x, w_rg, b_rg, w_ig, b_ig, lam):
    r = jax.nn.sigmoid(block_diag_linear(x, w_rg, b_rg).astype(jnp.float32))
    i = jax.nn.sigmoid(block_diag_linear(x, w_ig, b_ig).astype(jnp.float32))
    log_a = -LRU_C * r * jax.nn.softplus(-lam.astype(jnp.float32))
    a = jnp.exp(log_a)
    mult = jnp.sqrt(jnp.maximum(-jnp.expm1(2.0 * log_a), 0.0))
    b = mult * i * x.astype(jnp.float32)

    def combine(c1, c2):
        a1, b1 = c1
        a2, b2 = c2
        return a1 * a2, a2 * b1 + b2

    _, h = lax.associative_scan(combine, (a, b), axis=1)
    return h.astype(x.dtype)


def hybrid_mixer(n, w_in, conv_a_w, w_out_a, rnn_conv_w, rnn_conv_b,
                 w_rg, b_rg, w_ig, b_ig, lru_lambda, w_out_b, w_o):
    proj = n @ w_in
    a_x, a_b, a_c, r_x, r_y, g_a, g_b = jnp.split(proj, SPLITS, axis=-1)
    y_a = (a_b * causal_depthwise_conv(a_c * a_x, conv_a_w)) @ w_out_a
    r_c = causal_depthwise_conv(r_x, rnn_conv_w) + rnn_conv_b
    r_h = rg_lru(r_c, w_rg, b_rg, w_ig, b_ig, lru_lambda)
    y_b = (jax.nn.gelu(r_y, approximate=True) * r_h) @ w_out_b
    merged = jax.nn.sigmoid(g_a) * y_a + jax.nn.sigmoid(g_b) * y_b
    return merged @ w_o


def peer(n, w_q, sub_keys_1, sub_keys_2, expert_u, expert_v):
    bsz, s, d = n.shape
    xt = n.reshape(-1, PEER_CHUNK, d)

    def chunk(xc):
        q = (xc @ w_q).reshape(PEER_CHUNK, PEER_HEADS, 2, PEER_HALF)
        s1 = jnp.einsum('thd,hnd->thn', q[:, :, 0], sub_keys_1).astype(jnp.float32)
        s2 = jnp.einsum('thd,hnd->thn', q[:, :, 1], sub_keys_2).astype(jnp.float32)
        v1, i1 = lax.top_k(s1, PEER_TOPK)
        v2, i2 = lax.top_k(s2, PEER_TOPK)
        cand_s = (v1[..., :, None] + v2[..., None, :]).reshape(PEER_CHUNK, PEER_HEADS, PEER_TOPK * PEER_TOPK)
        cand_i = (i1[..., :, None] * PEER_NKEYS + i2[..., None, :]).reshape(PEER_CHUNK, PEER_HEADS, PEER_TOPK * PEER_TOPK)
        top_s, pos = lax.top_k(cand_s, PEER_TOPK)
        idx = jnp.take_along_axis(cand_i, pos, axis=-1)
        g = jax.nn.softmax(top_s, axis=-1)
        u_sel = jnp.take(expert_u, idx, axis=0)
        act = jax.nn.gelu(jnp.einsum('thkd,td->thk', u_sel, xc).astype(jnp.float32))
        v_sel = jnp.take(expert_v, idx, axis=0)
        return jnp.einsum('thk,thkd->td', (g * act).astype(xc.dtype), v_sel)

    return lax.map(chunk, xt).reshape(bsz, s, d)


def setup_inputs(seed: int = 0) -> dict:
    key = jax.random.key(seed)
    ks = jax.random.split(key, 24)
    f = jnp.float32
    L = DEPTH

    def nrm(k, shape, scale):
        return jax.random.normal(k, shape, f) * scale

    u = jax.random.uniform(ks[10], (L, D_RNN), f, 0.9, 0.999)
    a0 = u ** (1.0 / LRU_C)
    lru_lambda = jnp.log(a0) - jnp.log1p(-a0)
    return {
        "x": nrm(ks[0], (BATCH, SEQ, D_MODEL), 1.0),
        "norm_mix": 1.0 + nrm(ks[1], (L, D_MODEL), 0.01),
        "w_in": nrm(ks[2], (L, D_MODEL, W_IN_COLS), D_MODEL ** -0.5),
        "conv_a_w": nrm(ks[3], (L, CONV_WIDTH, D_CONV), CONV_WIDTH ** -0.5),
        "w_out_a": nrm(ks[4], (L, D_CONV, D_MODEL), D_CONV ** -0.5),
        "rnn_conv_w": nrm(ks[5], (L, RNN_CONV_WIDTH, D_RNN), RNN_CONV_WIDTH ** -0.5),
        "rnn_conv_b": nrm(ks[6], (L, D_RNN), 0.01),
        "w_rg": nrm(ks[7], (L, RNN_BLOCKS, RNN_BLOCK, RNN_BLOCK), RNN_BLOCK ** -0.5),
        "b_rg": nrm(ks[8], (L, D_RNN), 0.01),
        "w_ig": nrm(ks[9], (L, RNN_BLOCKS, RNN_BLOCK, RNN_BLOCK), RNN_BLOCK ** -0.5),
        "b_ig": nrm(ks[11], (L, D_RNN), 0.01),
        "lru_lambda": lru_lambda,
        "w_out_b": nrm(ks[12], (L, D_RNN, D_MODEL), D_RNN ** -0.5),
        "w_o": nrm(ks[13], (L, D_MODEL, D_MODEL), D_MODEL ** -0.5),
        "norm_ffn": 1.0 + nrm(ks[14], (L, D_MODEL), 0.01),
        "w_q": nrm(ks[15], (L, D_MODEL, PEER_HEADS * PEER_QDIM), D_MODEL ** -0.5),
        "sub_keys_1": nrm(ks[16], (L, PEER_HEADS, PEER_NKEYS, PEER_HALF), PEER_HALF ** -0.5),
        "sub_keys_2": nrm(ks[17], (L, PEER_HEADS, PEER_NKEYS, PEER_HALF), PEER_HALF ** -0.5),
        "expert_u": nrm(ks[18], (L, PEER_NEXPERTS, D_MODEL), D_MODEL ** -0.5),
        "expert_v": nrm(ks[19], (L, PEER_NEXPERTS, D_MODEL), PEER_HEADS ** -0.5),
        "norm_final": 1.0 + nrm(ks[20], (D_MODEL,), 0.01),
    }


def reference(x, norm_mix, w_in, conv_a_w, w_out_a, rnn_conv_w, rnn_conv_b,
              w_rg, b_rg, w_ig, b_ig, lru_lambda, w_out_b, w_o, norm_ffn,
              w_q, sub_keys_1, sub_keys_2, expert_u, expert_v, norm_final):
    h = x
    for l in range(DEPTH):
        n = rms_norm(h, norm_mix[l])
        h = h + hybrid_mixer(n, w_in[l], conv_a_w[l], w_out_a[l], rnn_conv_w[l],
                             rnn_conv_b[l], w_rg[l], b_rg[l], w_ig[l], b_ig[l],
                             lru_lambda[l], w_out_b[l], w_o[l])
        n = rms_norm(h, norm_ffn[l])
        h = h + peer(n, w_q[l], sub_keys_1[l], sub_keys_2[l], expert_u[l], expert_v[l])
    return rms_norm(h, norm_final)
```

```python
import threading
import numpy as np
import concourse.bass as bass
import concourse.mybir as mybir
from concourse.bass_utils import run_bass_kernel_spmd

F32 = mybir.dt.float32
BF16 = mybir.dt.bfloat16
I32 = mybir.dt.int32
AF = mybir.ActivationFunctionType
ALU = mybir.AluOpType
AX = mybir.AxisListType

D = 1024
KC = 8
RC = 10
NE = 16384
EPS = 1e-6
HALO = 4
WIN = 7680


class Buf:
    __slots__ = ("name", "w", "r", "waw_ok")

    def __init__(self, name):
        self.name = name
        self.w = None
        self.r = []
        self.waw_ok = False


class FW:
    ENGS = ("pe", "act", "dve", "pool", "sp")

    def __init__(self, nc):
        self.nc = nc
        self.eng = {"pe": nc.tensor, "act": nc.scalar, "dve": nc.vector,
                    "pool": nc.gpsimd, "sp": nc.sync}
        self.sems = {}
        self.cnt = {}
        self.seen = {e: {} for e in self.ENGS}
        for e in self.ENGS:
            self._mksem("E_" + e)
        self.n_wait = 0
        self.n_inst = 0
        self.tick_cb = None
        self.vt = {e: 0.0 for e in self.ENGS}
        self.tfin = {}
        self.last_start = 0.0
        self.dep_t = 0.0

    def _mksem(self, key):
        s = self.nc.alloc_semaphore(key)
        self.sems[key] = s
        self.cnt[key] = 0
        return s

    def _waits(self, e, reads, writes, is_dma=False):
        need = {}
        own = "E_" + e

        def add(dep, is_raw, waw=False):
            if dep is None:
                return
            k, v = dep
            if k == own and not is_dma:
                if e in ("pe", "sp") or waw:
                    return
            if need.get(k, 0) < v:
                need[k] = v
        for b in reads:
            add(b.w, True)
        for b in writes:
            add(b.w, False, b.waw_ok)
            for d in b.r:
                add(d, False)
        tf = self.tfin
        self.dep_t = max([tf.get(kv, 0.0) for kv in need.items()] + [0.0])
        seen = self.seen[e]
        out = []
        for k, v in need.items():
            if seen.get(k, 0) >= v:
                continue
            seen[k] = v
            out.append((k, v))
        return out

    COST = {"pe": (0.006, 1.0 / 2300.0), "act": (0.2, 1.0 / 1150.0), "dve": (0.07, 1.0 / 900.0),
            "pool": (0.3, 1.0 / 300.0), "sp": (0.05, 0.0)}

    def op(self, e, fn, reads=(), writes=(), sig=True, n=256):
        eng = self.eng[e]
        for k, v in self._waits(e, reads, writes):
            eng.wait_ge(self.sems[k], v)
            self.n_wait += 1
        c0, c1 = self.COST[e]
        t_start = max(self.vt[e], self.dep_t)
        t_fin = t_start + c0 + c1 * n
        self.vt[e] = t_fin
        self.last_start = t_start
        ins = fn(eng)
        self.n_inst += 1
        own = "E_" + e
        if sig:
            self.cnt[own] += 1
            ins.then_inc(self.sems[own], 1)
            v = self.cnt[own]
        else:
            v = self.cnt[own] + 1
        dep = (own, v)
        self.tfin[dep] = t_fin
        for b in writes:
            b.w = dep
            b.r = []
        for b in reads:
            if not b.r or b.r[-1] != dep:
                b.r.append(dep)
        if self.tick_cb:
            self.tick_cb(e, sig)
        return ins

    def dma(self, out, in_, reads=(), writes=(), q="sp", n=4096, **kw):
        eng = self.eng[q]
        for k, v in self._waits(q, reads, writes, is_dma=True):
            eng.wait_ge(self.sems[k], v)
            self.n_wait += 1
        t_start = max(self.vt[q], self.dep_t)
        self.vt[q] = t_start + (0.05 if q == "sp" else 1.0)
        self.last_start = t_start
        t_fin = t_start + 2.5 + n * 128 / 2.0e5
        tgt = (writes[0] if writes else reads[0])
        key = "D_" + q + "_" + tgt.name
        if key not in self.sems:
            self._mksem(key)
        ins = eng.dma_start(out=out, in_=in_, **kw)
        self.cnt[key] += 16
        ins.then_inc(self.sems[key], 16)
        dep = (key, self.cnt[key])
        self.tfin[dep] = t_fin
        for b in writes:
            b.w = dep
            b.r = []
        for b in reads:
            b.r.append(dep)
        if self.tick_cb:
            self.tick_cb(q, True)
        return ins

    def barrier(self):
        snap = dict(self.cnt)
        for e in self.ENGS:
            eng = self.eng[e]
            seen = self.seen[e]
            for k, v in snap.items():
                if v == 0 or k == "E_" + e:
                    continue
                if seen.get(k, 0) >= v:
                    continue
                seen[k] = v
                eng.wait_ge(self.sems[k], v)
                self.n_wait += 1

    def wait_all(self, e, bufs):
        eng = self.eng[e]
        need = {}
        for b in bufs:
            for d in ([b.w] if b.w else []) + b.r:
                k, v = d
                if need.get(k, 0) < v:
                    need[k] = v
        for k, v in need.items():
            eng.wait_ge(self.sems[k], v)


P_NMIX = 0
P_CAW = 8
P_RCW = 32
P_RCB = 72
P_BRG = 82
P_BIG = 92
P_LAM = 102
P_NFFN = 112
P_FLAG = 120
NPAR = 128


class Co:
    def __init__(self, fn):
        self.done = False
        self.run = threading.Semaphore(0)
        self.back = threading.Semaphore(0)
        self.err = None

        def target():
            self.run.acquire()
            try:
                fn()
            except BaseException as ex:
                self.err = ex
            self.done = True
            self.back.release()
        self.thread = threading.Thread(target=target, daemon=True)
        self.thread.start()

    def step(self):
        if self.done:
            return False
        self.run.release()
        self.back.acquire()
        if self.err is not None:
            raise self.err
        return not self.done

    def tick(self):
        if threading.current_thread() is self.thread:
            self.back.release()
            self.run.acquire()


def build_program(NT=4096, NPREV=4096, TB=256, stop=99):
    assert NT % TB == 0 and NPREV % TB == 0 and TB == 256
    TBH = TB + HALO
    NSUB = TB // 128
    NBLK = NT // TB
    nc = bass.Bass("TRN2", target_bir_lowering=False)
    f = FW(nc)
    co_box = [None]

    atomic = [0]

    def tick_cb(e, sig):
        co = co_box[0]
        if co is not None and (e != "pe" or sig) and not atomic[0]:
            co.tick()
    f.tick_cb = tick_cb

    def din(name, shape, dt=F32):
        return nc.dram_tensor(name, list(shape), dt, kind="ExternalInput").ap()

    xT_d = din("xT", [D, NT])
    xpT_d = din("xpT", [D, NPREV])
    par_d = din("params", [128, NPAR])
    gfin_d = din("gfin", [128, D])
    ident_d = din("ident", [128, 128])
    w_in_d = din("w_in", [D, WIN])
    w_oa_d = din("w_out_a", [D, D])
    w_ob_d = din("w_out_b", [1280, D])
    w_o_d = din("w_o", [D, D])
    w_q_d = din("w_q", [D, 2048])
    ut_d = din("UT", [D, NE])
    v_d = din("V", [NE, D])
    k1_d = din("K1T", [128, 1024])
    k2_d = din("K2T", [128, 1024])
    wrg_d = din("wrg", [128, 1280])
    wig_d = din("wig", [128, 1280])
    out_d = nc.dram_tensor("out", [NT, D], F32, kind="ExternalOutput").ap()

    def dscr(name, shape):
        return nc.dram_tensor(name, list(shape), BF16, kind="Internal").ap()

    w_in_b = dscr("w_in_b", [D, WIN])
    w_oa_b = dscr("w_oa_b", [D, D])
    w_ob_b = dscr("w_ob_b", [1280, D])
    w_o_b = dscr("w_o_b", [D, D])
    w_q_b = dscr("w_q_b", [D, 2048])
    ut_bt = dscr("ut_bt", [64, 128, KC * 256])
    v_b = dscr("v_b", [NE, D])
    k1_b = dscr("k1_b", [128, 1024])
    k2_b = dscr("k2_b", [128, 1024])
    wrg_b = dscr("wrg_b", [128, 1280])
    wig_b = dscr("wig_b", [128, 1280])

    def sb(name, shape, dt):
        return nc.alloc_sbuf_tensor(name, list(shape), dt).ap()

    B = {}

    def bf(name):
        if name not in B:
            B[name] = Buf(name)
        return B[name]

    par = sb("par_s", [128, NPAR], F32)
    gfin = sb("gfin_s", [128, D], F32)
    ident = sb("ident_s", [128, 128], F32)
    ones_bf = sb("ones_bf", [128, 128], BF16)
    cch = sb("cch", [128, 4 * RC], F32)
    wrg = sb("wrg_s", [128, 1280], BF16)
    wig = sb("wig_s", [128, 1280], BF16)
    kt_s = sb("kt_s", [128, 2048], BF16)
    iota128_i = sb("iota128_i", [128, 128], I32)
    iota256_i = sb("iota256_i", [128, 256], I32)
    iota16_f = sb("iota16_f", [128, 16], F32)
    iota_bf = sb("iota_bf", [128, 128], BF16)
    icst = sb("icst", [128, 8], I32)
    dummy = sb("dummy_s", [128, 8], F32)
    xbuf = sb("xbuf", [128, KC, TB], F32)
    nT = [sb("nT%d" % i, [128, KC, TBH], BF16) for i in range(2)]
    n2T = [sb("n2T%d" % i, [128, KC, TB], BF16) for i in range(2)]
    hstate = sb("hstate", [128, RC], F32)
    Wb = sb("Wb", [128, 128, TB], BF16)
    idxT = sb("idxT", [128, 3, TB], BF16)
    T16s = sb("T16s", [128, NSUB, 128], F32)
    smis = sb("smis", [128, NSUB, 2, 128], F32)
    uring = [sb("uring%d" % i, [128, KC, 256], BF16) for i in range(3)]
    vring = [sb("vring%d" % i, [128, 2, D], BF16) for i in range(3)]
    gA = [sb("gA%d" % i, [128, TB], BF16) for i in range(4)]
    Mb = [sb("Mb%d" % i, [128, TB], BF16) for i in range(6)]
    osb = sb("osb", [128, D], F32)
    junk = sb("junk", [128, 512], BF16)
    ssf = sb("ssf", [128, 8], F32)
    wring = [sb("wring%d" % i, [128, RC * 256], BF16) for i in range(2)]
    A1 = 54 * 1024
    arena = nc.alloc_sbuf_tensor("arena", [128, A1], mybir.dt.uint8)
    aoff = [0]

    def carve_at(off, shape, dt):
        n = int(np.prod(shape[1:])) * mybir.dt.size(dt)
        assert off + n <= A1, (off, n)
        ap = arena.ap()[:, off:off + n].bitcast(dt)
        if len(shape) == 3:
            ap = ap.rearrange("p (a b) -> p a b", a=shape[1])
        elif len(shape) == 4:
            ap = ap.rearrange("p (a b c) -> p a b c", a=shape[1], b=shape[2])
        return ap

    def carve(shape, dt):
        n = int(np.prod(shape[1:])) * mybir.dt.size(dt)
        off = aoff[0]
        aoff[0] += (n + 31) // 32 * 32
        return off, carve_at(off, shape, dt)

    oG1, G1 = carve([128, RC, TB], F32)
    oG2, G2 = carve([128, RC, TB], F32)
    oA, sA = carve([128, RC, TBH], BF16)
    oB, sB = carve([128, RC, TBH], BF16)
    oC, sC = carve([128, RC, TBH], BF16)
    oD, sD = carve([128, RC, TB], BF16)
    oE, sE = carve([128, KC, TB], BF16)
    oF, sF = carve([128, KC, TB], BF16)
    _, htmp0 = carve([128, TB], F32)
    _, htmp1 = carve([128, TB], F32)
    htmp = [htmp0, htmp1]
    _, rstd = carve([128, TB], F32)
    _, tmpa0 = carve([128, TB], F32)
    _, tmpa1 = carve([128, TB], F32)
    tmpa = [tmpa0, tmpa1]
    qT = carve_at(oG1, [128, 16, TB], BF16)
    Sx = carve_at(oG2, [128, 2048], F32)
    Sy = carve_at(oA, [128, 2048], F32)
    o = oC
    V16 = carve_at(o, [128, 16, 16], F32); o += 1024
    idxi = carve_at(o, [128, 256], I32); o += 1024
    idxf = carve_at(o, [128, 256], F32); o += 1024
    posi = carve_at(o, [128, 3, 128], I32); o += 1536
    posf = carve_at(o, [128, 2, 128], F32); o += 1024
    smi = carve_at(o, [128, 2, 128], F32); o += 1024
    T16 = carve_at(o, [128, 8, 16], F32); o += 512
    assert o <= oE
    SMALL = [bf("sC"), bf("sD")]
    Loh = [carve_at(oG1, [128, 16, 128], BF16), carve_at(oG2 + 4096, [128, 16, 128], BF16)]
    R1oh = [carve_at(oG1 + 4096, [128, 16, 128], BF16), carve_at(oA, [128, 16, 128], BF16)]
    Roh = [carve_at(oG2, [128, 16, 128], BF16), carve_at(oA + 4096, [128, 16, 128], BF16)]
    wsm = carve_at(oC, [128, 2, 128], F32)
    wzs = carve_at(oC + 1024, [128, 16], F32)

    ps = nc.alloc_psum_tensor("ps", [128, 8, 512], F32).ap()
    PSB = [bf("psb%d" % i) for i in range(8)]
    st = {"wslot": 0, "psrot": 0}
    S1Q = "sp"
    st["banks"] = (0, 1, 2, 3, 4, 5, 6, 7)

    def next_ps():
        S1BANKS = st["banks"]
        b = S1BANKS[st["psrot"] % len(S1BANKS)]
        st["psrot"] += 1
        return b

    f.dma(par, par_d, writes=[bf("par")])
    f.dma(gfin, gfin_d, writes=[bf("gfin")])
    f.dma(ident, ident_d, writes=[bf("ident")])

    deferred = []
    defer_on = [False]

    def cdma(dst, src, name):
        if defer_on[0]:
            bf(name)
            deferred.append((dst, src, name))
        else:
            f.dma(dst, src, writes=[bf(name)], q="pool")

    def conv(dst, src, name, rows_per, c0=None, c1=None):
        R = src.shape[0]
        for r0 in range(0, R, rows_per):
            if c0 is None:
                cdma(dst[r0:r0 + rows_per, :], src[r0:r0 + rows_per, :], name)
            else:
                cdma(dst[r0:r0 + rows_per, c0:c1], src[r0:r0 + rows_per, c0:c1], name)

    def emit_deferred(k):
        for _ in range(min(k, len(deferred))):
            dst, src, name = deferred.pop(0)
            f.dma(dst, src, writes=[bf(name)], q="pool")

    conv(wrg_b, wrg_d, "wrg_b", 128)
    conv(wig_b, wig_d, "wig_b", 128)
    conv(w_in_b, w_in_d, "w_in_b_rx", 256, 3072, 4352)
    f.dma(wrg, wrg_b, reads=[bf("wrg_b")], writes=[bf("wrg")])
    f.dma(wig, wig_b, reads=[bf("wig_b")], writes=[bf("wig")])
    defer_on[0] = True
    conv(w_in_b, w_in_d, "w_in_b", 256, 0, 3072)
    conv(w_in_b, w_in_d, "w_in_b", 256, 4352, WIN)
    conv(w_oa_b, w_oa_d, "w_oa_b", 256)
    conv(w_ob_b, w_ob_d, "w_ob_b", 256)
    conv(w_o_b, w_o_d, "w_o_b", 256)
    conv(w_q_b, w_q_d, "w_q_b", 256)
    conv(k1_b, k1_d, "k1_b", 128)
    conv(k2_b, k2_d, "k2_b", 128)
    for k in range(KC):
        for gh in range(4):
            g0 = gh * 16
            cdma(ut_bt[g0:g0 + 16, :, k * 256:(k + 1) * 256].rearrange("g p c -> p g c"),
                 ut_d[k * 128:(k + 1) * 128, g0 * 256:(g0 + 16) * 256].rearrange("p (g c) -> p g c", c=256), "ut_b")
    conv(v_b, v_d, "v_b", 1024)
    defer_on[0] = False
    n_def = len(deferred)
    per_blk = (n_def + (NPREV // TB) - 1) // (NPREV // TB)

    CON = bf("consts")
    f.op("pool", lambda e: e.iota(iota128_i, pattern=[[1, 128]], base=0, channel_multiplier=0), writes=[CON])
    f.op("pool", lambda e: e.iota(iota256_i, pattern=[[1, 256]], base=0, channel_multiplier=0), writes=[CON])
    f.op("pool", lambda e: e.iota(iota16_f, pattern=[[1, 16]], base=0, channel_multiplier=0,
                                  allow_small_or_imprecise_dtypes=True), writes=[CON])
    f.op("pool", lambda e: e.iota(iota_bf, pattern=[[1, 128]], base=0, channel_multiplier=0,
                                  allow_small_or_imprecise_dtypes=True), writes=[CON])
    for i, val in enumerate([-128, -256, 127, 255, 15, 4]):
        f.op("pool", lambda e, i=i, val=val: e.memset(icst[:, i:i + 1], val), writes=[CON])
    f.op("pool", lambda e: e.memset(ones_bf, 1.0), writes=[CON])
    f.op("pool", lambda e: e.memset(dummy, 0.0), writes=[bf("dummy")])
    f.op("act", lambda e: e.activation(out=cch[:, 0:RC], in_=par[:, P_LAM:P_LAM + RC], func=AF.Exp, scale=-1.0),
         reads=[bf("par")], writes=[bf("cch")])
    f.op("act", lambda e: e.activation(out=cch[:, 0:RC], in_=cch[:, 0:RC], func=AF.Ln, bias=1.0, scale=1.0),
         reads=[bf("cch")], writes=[bf("cch")])
    f.op("dve", lambda e: e.tensor_scalar(out=cch[:, RC:2 * RC], in0=cch[:, 0:RC], scalar1=-4.0, scalar2=None,
                                          op0=ALU.mult), reads=[bf("cch")], writes=[bf("cchb")])
    f.op("dve", lambda e: e.tensor_scalar(out=cch[:, 0:RC], in0=cch[:, 0:RC], scalar1=-8.0, scalar2=None,
                                          op0=ALU.mult), reads=[bf("cch"), bf("cchb")], writes=[bf("cch")])
    f.op("dve", lambda e: e.tensor_scalar(out=cch[:, 2 * RC:4 * RC], in0=par[:, P_BRG:P_BRG + 2 * RC], scalar1=0.5,
                                          scalar2=None, op0=ALU.mult), reads=[bf("par")], writes=[bf("cchb")])
    CCH = [bf("cch"), bf("cchb")]

    def finish():
        f.barrier()
        return nc, f
    if stop == 0:
        return finish()

    def wsrc_buf(src, col0):
        if src is w_in_b:
            return bf("w_in_b_rx") if 3072 <= col0 < 4352 else bf("w_in_b")
        return bf({id(w_oa_b): "w_oa_b", id(w_ob_b): "w_ob_b", id(w_o_b): "w_o_b", id(w_q_b): "w_q_b"}[id(src)])

    def load_w(src, col0, ncols, K):
        i = st["wslot"] % 2
        st["wslot"] += 1
        dst = wring[i][:, 0:K * ncols].rearrange("p (k c) -> p k c", k=K)
        f.dma(dst, src[:, col0:col0 + ncols].rearrange("(k p) c -> p k c", p=128),
              reads=[wsrc_buf(src, col0)], writes=[bf("wring%d" % i)], q=S1Q, n=K * ncols * 2)
        return i, dst

    def proj(src, col0, nchunks, K, rhs_fn, ncols_rhs, rhs_bufs, evac):
        loads = [(c0, min(2, nchunks - c0)) for c0 in range(0, nchunks, 2)]
        slots = {}
        slots[0] = load_w(src, col0 + loads[0][0] * 128, loads[0][1] * 128, K)
        for li, (c0, n) in enumerate(loads):
            if li + 1 < len(loads):
                slots[li + 1] = load_w(src, col0 + loads[li + 1][0] * 128, loads[li + 1][1] * 128, K)
            sl, wv = slots[li]
            for cl in range(n):
                ci = c0 + cl
                b = next_ps()
                pso = ps[:, b, 0:ncols_rhs]
                for k in range(K):
                    f.op("pe", lambda e, k=k, cl=cl, wv=wv, pso=pso: e.matmul(
                        pso, wv[:, k, cl * 128:(cl + 1) * 128], rhs_fn(k), start=(k == 0), stop=(k == K - 1)),
                        reads=[bf("wring%d" % sl)] + rhs_bufs, writes=[PSB[b]], sig=(k == K - 1))
                evac(ci, pso, PSB[b])

    class BSet:
        def __init__(self, suffix, **kw):
            self.suffix = suffix
            self.__dict__.update(kw)

        def b(self, name):
            return bf(name + self.suffix)

    BS0 = BSet("", sE=sE, sB=sB, sA=sA, G1=G1, G2=G2, sD=sD, rstd=rstd, htmp=htmp)
    Wb_flat = Wb.rearrange("p a b -> p (a b)")

    def carve_w(off, shape, dt):
        n = int(np.prod(shape[1:])) * mybir.dt.size(dt)
        assert off % 4 == 0 and off + n <= 65536
        ap = Wb_flat[:, off // 2:(off + n) // 2].bitcast(dt)
        if len(shape) == 3:
            ap = ap.rearrange("p (a b) -> p a b", a=shape[1])
        return ap, off + (n + 31) // 32 * 32

    _o = KC * 1280 * 2
    _G1, _o = carve_w(_o, [128, RC, TB], F32)
    _G2, _o = carve_w(_o, [128, RC, TB], F32)
    _sA, _o = carve_w(_o, [128, RC, TBH], BF16)
    _sB, _o = carve_w(_o, [128, RC, TBH], BF16)
    _sD, _o = carve_w(_o, [128, RC, TB], BF16)
    _sE, _o = carve_w(_o, [128, KC, TB], BF16)
    _h0, _o = carve_w(_o, [128, TB], F32)
    _h1, _o = carve_w(_o, [128, TB], F32)
    _rs, _o = carve_w(_o, [128, TB], F32)
    BS1 = BSet("_1", sE=_sE, sB=_sB, sA=_sA, G1=_G1, G2=_G2, sD=_sD, rstd=_rs, htmp=[_h0, _h1])

    def rmsnorm_T(src, src_b, gcol, dst, dst_b, dst_off, bs=None):
        bs = bs or BS0
        for c in range(KC):
            f.op("act", lambda e, c=c: e.activation(out=bs.sE[:, c, :], in_=src[:, c, :], func=AF.Square),
                 reads=[src_b], writes=[bs.b("sE")])
        b = next_ps()
        pso = ps[:, b, 0:TB]
        for k in range(KC):
            f.op("pe", lambda e, k=k: e.matmul(pso, ones_bf, bs.sE[:, k, :], start=(k == 0), stop=(k == KC - 1)),
                 reads=[bs.b("sE"), CON], writes=[PSB[b]], sig=(k == KC - 1))
        atomic[0] += 1
        f.op("act", lambda e: e.activation(out=bs.rstd, in_=pso, func=AF.Ln, scale=1.0 / D, bias=EPS),
             reads=[PSB[b]], writes=[bs.b("rstd")])
        f.op("act", lambda e: e.activation(out=bs.rstd, in_=bs.rstd, func=AF.Exp, scale=-0.5),
             reads=[bs.b("rstd")], writes=[bs.b("rstd")])
        atomic[0] -= 1
        for c in range(KC):
            f.op("dve", lambda e, c=c: e.scalar_tensor_tensor(
                out=dst[:, c, dst_off:dst_off + TB], in0=src[:, c, :], scalar=par[:, gcol + c:gcol + c + 1],
                in1=bs.rstd, op0=ALU.mult, op1=ALU.mult), reads=[src_b, bs.b("rstd"), bf("par")], writes=[dst_b])

    wrx = Wb.rearrange("p a b -> p (a b)")[:, 0:KC * 1280].rearrange("p (k c) -> p k c", k=KC)

    def rbranch(nTc, nTb, main, bs=None, part="all"):
        bs = bs or BS0
        def ev_rx(ci, pso, pb):
            f.op("act", lambda e: e.activation(out=bs.sB[:, ci, :], in_=pso, func=AF.Copy), reads=[pb], writes=[bs.b("sB")])
        if main:
            proj(w_in_b, 3072, RC, KC, lambda k: nTc[:, k, :], TBH, [nTb], ev_rx)
        else:
            for ci in range(RC if part != "B" else 0):
                b = next_ps()
                pso = ps[:, b, 0:TBH]
                for k in range(KC):
                    f.op("pe", lambda e, k=k, ci=ci, pso=pso: e.matmul(
                        pso, wrx[:, k, ci * 128:(ci + 1) * 128], nTc[:, k, :], start=(k == 0), stop=(k == KC - 1)),
                        reads=[bf("Wb"), nTb], writes=[PSB[b]], sig=(k == KC - 1))
                ev_rx(ci, pso, PSB[b])
        if part == "A":
            return

        if not main:
            for n in range(RC):
                f.op("dve", lambda e, n=n: e.tensor_scalar(
                    out=bs.G1[:, n, :], in0=bs.sB[:, n, 1:1 + TB], scalar1=par[:, P_RCW + n:P_RCW + n + 1],
                    scalar2=par[:, P_RCB + n:P_RCB + n + 1], op0=ALU.mult, op1=ALU.add),
                    reads=[bs.b("sB"), bf("par")], writes=[bs.b("G1")])
                for j in range(1, 4):
                    dst = bs.sA[:, n, 0:TB] if j == 3 else bs.G1[:, n, :]
                    f.op("dve", lambda e, n=n, j=j, dst=dst: e.scalar_tensor_tensor(
                        out=dst, in0=bs.sB[:, n, 1 + j:1 + j + TB], scalar=par[:, P_RCW + j * RC + n:P_RCW + j * RC + n + 1],
                        in1=bs.G1[:, n, :], op0=ALU.mult, op1=ALU.add),
                        reads=[bs.b("sB"), bf("par"), bs.b("G1")], writes=[bs.b("sA") if j == 3 else bs.b("G1")])
        if main:
            for n in range(0, RC):
                ht = bs.htmp[n % 2]
                hb = bs.b("htmp%d" % (n % 2))
                f.op("dve", lambda e, n=n, ht=ht: e.tensor_scalar(
                    out=ht, in0=bs.sB[:, n, 1:1 + TB], scalar1=par[:, P_RCW + n:P_RCW + n + 1],
                    scalar2=par[:, P_RCB + n:P_RCB + n + 1], op0=ALU.mult, op1=ALU.add),
                    reads=[bs.b("sB"), bf("par")], writes=[hb])
                for j in range(1, 4):
                    dst = bs.sA[:, n, 0:TB] if j == 3 else ht
                    f.op("dve", lambda e, n=n, j=j, dst=dst, ht=ht: e.scalar_tensor_tensor(
                        out=dst, in0=bs.sB[:, n, 1 + j:1 + j + TB], scalar=par[:, P_RCW + j * RC + n:P_RCW + j * RC + n + 1],
                        in1=ht, op0=ALU.mult, op1=ALU.add),
                        reads=[bs.b("sB"), bf("par"), hb], writes=[bs.b("sA") if j == 3 else hb])
        for hf in range(0):
            n0 = hf * 5
            g1 = bs.G1[:, 0:5, :]
            g2 = bs.G2[:, 0:5, :]

            def wv(j):
                return par[:, P_RCW + j * RC + n0:P_RCW + j * RC + n0 + 5].unsqueeze(2).to_broadcast([128, 5, TB])
            f.op("pool", lambda e: e.tensor_tensor(out=g1, in0=bs.sB[:, n0:n0 + 5, 1:1 + TB], in1=wv(0), op=ALU.mult),
                 reads=[bs.b("sB"), bf("par")], writes=[bs.b("G1")], n=1280)
            for j in range(1, 4):
                f.op("pool", lambda e, j=j: e.tensor_tensor(out=g2, in0=bs.sB[:, n0:n0 + 5, 1 + j:1 + j + TB], in1=wv(j),
                                                            op=ALU.mult), reads=[bs.b("sB"), bf("par")], writes=[bs.b("G2")], n=1280)
                f.op("pool", lambda e: e.tensor_tensor(out=g1, in0=g1, in1=g2, op=ALU.add),
                     reads=[bs.b("G1"), bs.b("G2")], writes=[bs.b("G1")], n=1280)
            f.op("pool", lambda e: e.tensor_tensor(
                out=bs.sA[:, n0:n0 + 5, 0:TB], in0=g1,
                in1=par[:, P_RCB + n0:P_RCB + n0 + 5].unsqueeze(2).to_broadcast([128, 5, TB]), op=ALU.add),
                reads=[bs.b("G1"), bf("par")], writes=[bs.b("sA")], n=1280)
        for n in range(RC):
            b = next_ps()
            pso = ps[:, b, 0:TB]
            f.op("pe", lambda e, n=n, pso=pso: e.matmul(pso, wrg[:, n * 128:(n + 1) * 128], bs.sA[:, n, 0:TB],
                                                        start=True, stop=True),
                 reads=[bs.b("sA"), bf("wrg")], writes=[PSB[b]])
            f.op("act", lambda e, n=n, pso=pso: e.activation(out=bs.G1[:, n, :], in_=pso, func=AF.Tanh,
                                                             bias=cch[:, 2 * RC + n:2 * RC + n + 1], scale=0.5),
                 reads=[PSB[b]] + CCH, writes=[bs.b("G1")])
            b2 = next_ps()
            pso2 = ps[:, b2, 0:TB]
            f.op("pe", lambda e, n=n, pso2=pso2: e.matmul(pso2, wig[:, n * 128:(n + 1) * 128], bs.sA[:, n, 0:TB],
                                                          start=True, stop=True),
                 reads=[bs.b("sA"), bf("wig")], writes=[PSB[b2]])
            f.op("act", lambda e, n=n, pso2=pso2: e.activation(out=bs.sD[:, n, :], in_=pso2, func=AF.Tanh,
                                                               bias=cch[:, 3 * RC + n:3 * RC + n + 1], scale=0.5),
                 reads=[PSB[b2]] + CCH, writes=[bs.b("sD")])
        atomic[0] += 1
        for n in range(RC):
            f.op("act", lambda e, n=n: e.activation(out=bs.G2[:, n, :], in_=bs.G1[:, n, :], func=AF.Exp,
                                                    scale=cch[:, n:n + 1], bias=cch[:, n:n + 1]),
                 reads=[bs.b("G1")] + CCH, writes=[bs.b("G2")])
        for n in range(RC):
            f.op("act", lambda e, n=n: e.activation(out=bs.G1[:, n, :], in_=bs.G1[:, n, :], func=AF.Exp,
                                                    scale=cch[:, RC + n:RC + n + 1], bias=cch[:, RC + n:RC + n + 1]),
                 reads=[bs.b("G1")] + CCH, writes=[bs.b("G1")])
        for hf in range(2):
            f.op("act", lambda e, hf=hf: e.activation(out=bs.G2[:, hf * 5:hf * 5 + 5, :], in_=bs.G2[:, hf * 5:hf * 5 + 5, :],
                                                      func=AF.Ln, scale=-1.0, bias=1.0000001),
                 reads=[bs.b("G2")], writes=[bs.b("G2")], n=1280)
        for hf in range(2):
            f.op("act", lambda e, hf=hf: e.activation(out=bs.G2[:, hf * 5:hf * 5 + 5, :], in_=bs.G2[:, hf * 5:hf * 5 + 5, :],
                                                      func=AF.Exp, scale=0.5),
                 reads=[bs.b("G2")], writes=[bs.b("G2")], n=1280)
        atomic[0] -= 1
        for hf in range(2):
            sl = slice(hf * 5, hf * 5 + 5)
            f.op("dve", lambda e, sl=sl: e.scalar_tensor_tensor(out=bs.G2[:, sl, :], in0=bs.sD[:, sl, :], scalar=1.0,
                                                                in1=bs.G2[:, sl, :], op0=ALU.add, op1=ALU.mult),
                 reads=[bs.b("G2"), bs.b("sD")], writes=[bs.b("G2")], n=1280)
            f.op("dve", lambda e, sl=sl: e.scalar_tensor_tensor(out=bs.G2[:, sl, :], in0=bs.G2[:, sl, :], scalar=0.5,
                                                                in1=bs.sA[:, sl, 0:TB], op0=ALU.mult, op1=ALU.mult),
                 reads=[bs.b("G2"), bs.b("sA")], writes=[bs.b("G2")], n=1280)
        for n in range(RC):
            ht = bs.htmp[n % 2]
            hb = bs.b("htmp%d" % (n % 2))
            f.op("dve", lambda e, n=n, ht=ht: e.tensor_tensor_scan(out=ht, data0=bs.G1[:, n, :], data1=bs.G2[:, n, :],
                                                                   initial=hstate[:, n:n + 1], op0=ALU.mult, op1=ALU.add),
                 reads=[bs.b("G1"), bs.b("G2"), bf("hstate")], writes=[hb])
            f.op("dve", lambda e, n=n, ht=ht: e.tensor_copy(out=hstate[:, n:n + 1], in_=ht[:, TB - 1:TB]),
                 reads=[hb], writes=[bf("hstate")])
            if main:
                f.op("dve", lambda e, n=n, ht=ht: e.tensor_tensor(out=bs.sB[:, n, 0:TB], in0=sC[:, n, 0:TB], in1=ht, op=ALU.mult),
                     reads=[hb, bf("sC")], writes=[bs.b("sB")])

    def halo_from(prev, cur):
        f.op("pool", lambda e: e.tensor_copy(out=nT[cur][:, :, 0:HALO], in_=nT[prev][:, :, TB:TB + HALO]),
             reads=[bf("nT%d" % prev)], writes=[bf("nT%d" % cur)])

    f.op("pool", lambda e: e.memset(nT[1][:, :, TB:TB + HALO], 0.0), writes=[bf("nT1")])
    f.op("pool", lambda e: e.memset(hstate, 0.0), writes=[bf("hstate")])
    cur = 0
    f.dma(wrx, w_in_b[:, 3072:4352].rearrange("(k p) c -> p k c", p=128), reads=[bf("w_in_b_rx")], writes=[bf("Wb")])
    NPB = NPREV // TB

    def stageA(blk):
        c = blk % 2
        bsx = BS0 if blk % 2 == 0 else BS1
        f.dma(xbuf, xpT_d[:, blk * TB:(blk + 1) * TB].rearrange("(k p) t -> p k t", p=128), writes=[bf("xbuf")])
        emit_deferred(per_blk)
        halo_from(1 - c, c)
        rmsnorm_T(xbuf, bf("xbuf"), P_NMIX, nT[c], bf("nT%d" % c), HALO, bsx)
        rbranch(nT[c], bf("nT%d" % c), False, bsx, part="A")

    def stageB(blk):
        c = blk % 2
        bsx = BS0 if blk % 2 == 0 else BS1
        rbranch(nT[c], bf("nT%d" % c), False, bsx, part="B")

    stageA(0)
    for blk in range(NPB):
        if blk + 1 < NPB:
            stageA(blk + 1)
        stageB(blk)
    cur = NPB % 2
    emit_deferred(len(deferred))
    f.op("pool", lambda e: e.memset(dummy[:, 1:2], 0.0),
         writes=[BS1.b(nm) for nm in ("sE", "sB", "sA", "G1", "G2", "sD", "rstd", "htmp0", "htmp1")] + [bf("Wb"), bf("dummy")])
    st["banks"] = (6, 7)
    if stop == 1:
        return finish()
    f.op("dve", lambda e: e.tensor_scalar(out=hstate, in0=hstate, scalar1=par[:, P_FLAG:P_FLAG + 1], scalar2=None,
                                          op0=ALU.mult), reads=[bf("hstate"), bf("par")], writes=[bf("hstate")])

    def mixer(blk, cur):
        nTc, nTb = nT[cur], bf("nT%d" % cur)
        n2c, n2b = n2T[blk % 2], bf("n2T%d" % (blk % 2))
        f.dma(xbuf, xT_d[:, blk * TB:(blk + 1) * TB].rearrange("(k p) t -> p k t", p=128), writes=[bf("xbuf")], q=S1Q, n=8192)
        halo_from(1 - cur, cur)
        rmsnorm_T(xbuf, bf("xbuf"), P_NMIX, nTc, nTb, HALO)
        def ev_ax(ci, pso, pb):
            f.op("act", lambda e: e.activation(out=sC[:, ci, :], in_=pso, func=AF.Copy), reads=[pb], writes=[bf("sC")])
        proj(w_in_b, 0, KC, KC, lambda k: nTc[:, k, :], TBH, [nTb], ev_ax)

        def ev_ac(ci, pso, pb):
            f.op("dve", lambda e: e.tensor_tensor(out=sA[:, ci, :], in0=pso, in1=sC[:, ci, :], op=ALU.mult),
                 reads=[pb, bf("sC")], writes=[bf("sA")])
        proj(w_in_b, 2048, KC, KC, lambda k: nTc[:, k, :], TBH, [nTb], ev_ac)

        def ev_ab(ci, pso, pb):
            f.op("act", lambda e: e.activation(out=sD[:, ci, :], in_=pso, func=AF.Copy), reads=[pb], writes=[bf("sD")])
        proj(w_in_b, 1024, KC, KC, lambda k: nTc[:, k, HALO:TBH], TB, [nTb], ev_ab)
        for n in range(KC):
            t = tmpa[n % 2]
            tb = bf("tmpa%d" % (n % 2))
            f.op("dve", lambda e, n=n, t=t: e.tensor_scalar(
                out=t, in0=sA[:, n, 2:2 + TB], scalar1=par[:, P_CAW + n:P_CAW + n + 1], scalar2=None, op0=ALU.mult),
                reads=[bf("sA"), bf("par")], writes=[tb])
            for j in range(1, 3):
                f.op("dve", lambda e, n=n, j=j, t=t: e.scalar_tensor_tensor(
                    out=t, in0=sA[:, n, 2 + j:2 + j + TB], scalar=par[:, P_CAW + j * KC + n:P_CAW + j * KC + n + 1],
                    in1=t, op0=ALU.mult, op1=ALU.add), reads=[bf("sA"), bf("par"), tb], writes=[tb])
            f.op("dve", lambda e, n=n, t=t: e.tensor_tensor(out=sC[:, n, 0:TB], in0=t, in1=sD[:, n, :], op=ALU.mult),
                 reads=[tb, bf("sD")], writes=[bf("sC")])
        def ev_ga(ci, pso, pb):
            f.op("act", lambda e: e.activation(out=sE[:, ci, :], in_=pso, func=AF.Tanh, scale=0.5), reads=[pb], writes=[bf("sE")])
        proj(w_in_b, 5632, KC, KC, lambda k: nTc[:, k, HALO:TBH], TB, [nTb], ev_ga)
        def ev_ya(ci, pso, pb):
            f.op("dve", lambda e: e.scalar_tensor_tensor(out=sF[:, ci, :], in0=sE[:, ci, :], scalar=1.0, in1=pso,
                                                         op0=ALU.add, op1=ALU.mult),
                 reads=[pb, bf("sE")], writes=[bf("sF")])
        proj(w_oa_b, 0, KC, KC, lambda k: sC[:, k, 0:TB], TB, [bf("sC")], ev_ya)
        def ev_ry(ci, pso, pb):
            f.op("act", lambda e: e.activation(out=sC[:, ci, 0:TB], in_=pso, func=AF.Gelu_apprx_tanh), reads=[pb], writes=[bf("sC")])
        proj(w_in_b, 4352, RC, KC, lambda k: nTc[:, k, HALO:TBH], TB, [nTb], ev_ry)
        rbranch(nTc, nTb, True)
        def ev_gb(ci, pso, pb):
            f.op("act", lambda e: e.activation(out=sD[:, ci, :], in_=pso, func=AF.Tanh, scale=0.5), reads=[pb], writes=[bf("sD")])
        proj(w_in_b, 6656, KC, KC, lambda k: nTc[:, k, HALO:TBH], TB, [nTb], ev_gb)
        def ev_yb(ci, pso, pb):
            t = tmpa[ci % 2]
            f.op("dve", lambda e: e.scalar_tensor_tensor(out=t, in0=sD[:, ci, :], scalar=1.0, in1=pso,
                                                         op0=ALU.add, op1=ALU.mult),
                 reads=[pb, bf("sD")], writes=[bf("tmpa%d" % (ci % 2))])
            f.op("dve", lambda e: e.tensor_tensor(out=sF[:, ci, :], in0=t, in1=sF[:, ci, :], op=ALU.add),
                 reads=[bf("tmpa%d" % (ci % 2)), bf("sF")], writes=[bf("sF")])
        proj(w_ob_b, 0, KC, RC, lambda k: sB[:, k, 0:TB], TB, [bf("sB")], ev_yb)
        def ev_o(ci, pso, pb):
            f.op("dve", lambda e: e.scalar_tensor_tensor(out=xbuf[:, ci, :], in0=pso, scalar=0.5, in1=xbuf[:, ci, :],
                                                         op0=ALU.mult, op1=ALU.add),
                 reads=[pb, bf("xbuf")], writes=[bf("xbuf")])
        proj(w_o_b, 0, KC, KC, lambda k: sF[:, k, :], TB, [bf("sF")], ev_o)
        rmsnorm_T(xbuf, bf("xbuf"), P_NFFN, n2c, n2b, 0)

    def select(blk):
        n2c, n2b = n2T[blk % 2], bf("n2T%d" % (blk % 2))

        def ev_q(ci, pso, pb):
            f.op("act", lambda e: e.activation(out=qT[:, ci, :], in_=pso, func=AF.Copy), reads=[pb], writes=[bf("G1")])
        proj(w_q_b, 0, 16, KC, lambda k: n2c[:, k, :], TB, [n2b], ev_q)
        kb = bf("kt_s")
        SxB, SyB = [bf("G2")], [bf("sA"), bf("sB")]
        Sx3 = Sx.rearrange("p (g n) -> p g n", g=16)
        Sy3 = Sy.rearrange("p (g n) -> p g n", g=16)
        for s in range(NSUB):
            for bg in range(4):
                b = next_ps()
                for gg in range(4):
                    g = bg * 4 + gg
                    h, p = g // 2, g % 2
                    f.op("pe", lambda e, g=g, gg=gg, b=b, h=h, p=p: e.matmul(
                        ps[:, b, gg * 128:(gg + 1) * 128], qT[:, g, s * 128:(s + 1) * 128],
                        kt_s[:, p * 1024 + h * 128:p * 1024 + (h + 1) * 128], start=True, stop=True),
                        reads=[bf("G1"), kb], writes=[PSB[b]], sig=(gg == 3))
                f.op("act", lambda e, b=b, bg=bg: e.activation(out=Sx[:, bg * 512:(bg + 1) * 512], in_=ps[:, b, :], func=AF.Copy),
                     reads=[PSB[b]], writes=SxB, n=512)
            f.op("dve", lambda e: e.scalar_tensor_tensor(
                out=Sx3.bitcast(I32), in0=Sx3.bitcast(I32), scalar=icst[:, 0:1],
                in1=iota128_i.unsqueeze(1).to_broadcast([128, 16, 128]), op0=ALU.bitwise_and, op1=ALU.bitwise_or),
                reads=SxB + [CON], writes=SxB, n=2048)
            for g in range(16):
                f.op("dve", lambda e, g=g: e.max(out=V16[:, g, 0:8], in_=Sx3[:, g, :]), reads=SxB, writes=SMALL)
                f.op("dve", lambda e, g=g: e.match_replace(out=Sy3[:, g, :], in_to_replace=V16[:, g, 0:8],
                                                           in_values=Sx3[:, g, :], imm_value=-1e30),
                     reads=SxB + SMALL, writes=SyB)
                f.op("dve", lambda e, g=g: e.max(out=V16[:, g, 8:16], in_=Sy3[:, g, :]), reads=SyB, writes=SMALL)
            V2 = V16.rearrange("p g k -> p (g k)")
            f.op("dve", lambda e: e.tensor_scalar(out=idxi, in0=V2.bitcast(I32), scalar1=icst[:, 2:3], scalar2=None,
                                                  op0=ALU.bitwise_and), reads=SMALL + [CON], writes=SMALL)
            f.op("dve", lambda e: e.tensor_copy(out=idxf, in_=idxi), reads=SMALL, writes=SMALL)
            V4 = V16.rearrange("p (h q) k -> p h q k", q=2)
            C4 = Sx.rearrange("p (h a b) -> p h a b", h=8, a=16)
            f.op("dve", lambda e: e.tensor_tensor(
                out=C4, in0=V4[:, :, 0, :].unsqueeze(3).to_broadcast([128, 8, 16, 16]),
                in1=V4[:, :, 1, :].unsqueeze(2).to_broadcast([128, 8, 16, 16]), op=ALU.add),
                reads=SMALL, writes=SxB, n=2048)
            Cx3 = Sx.rearrange("p (h n) -> p h n", h=8)
            Cy3 = Sy.rearrange("p (h n) -> p h n", h=8)
            f.op("dve", lambda e: e.scalar_tensor_tensor(
                out=Cy3.bitcast(I32), in0=Cx3.bitcast(I32), scalar=icst[:, 1:2],
                in1=iota256_i.unsqueeze(1).to_broadcast([128, 8, 256]), op0=ALU.bitwise_and, op1=ALU.bitwise_or),
                reads=SxB + [CON], writes=SyB, n=2048)
            for h in range(8):
                f.op("dve", lambda e, h=h: e.max(out=T16[:, h, 0:8], in_=Cy3[:, h, :]), reads=SyB, writes=SMALL)
                f.op("dve", lambda e, h=h: e.match_replace(out=Cx3[:, h, :], in_to_replace=T16[:, h, 0:8],
                                                           in_values=Cy3[:, h, :], imm_value=-1e30),
                     reads=SyB + SMALL, writes=SxB)
                f.op("dve", lambda e, h=h: e.max(out=T16[:, h, 8:16], in_=Cx3[:, h, :]), reads=SxB, writes=SMALL)
            T2d = T16.rearrange("p h k -> p (h k)")
            f.op("dve", lambda e: e.tensor_copy(out=T16s[:, s, :], in_=T2d), reads=SMALL, writes=[bf("T16s")])
            f.op("dve", lambda e: e.tensor_scalar(out=posi[:, 0, :], in0=T2d.bitcast(I32), scalar1=icst[:, 3:4],
                                                  scalar2=None, op0=ALU.bitwise_and), reads=SMALL + [CON], writes=SMALL)
            f.op("dve", lambda e: e.tensor_scalar(out=posi[:, 1, :], in0=posi[:, 0, :], scalar1=icst[:, 5:6],
                                                  scalar2=None, op0=ALU.logical_shift_right), reads=SMALL + [CON], writes=SMALL)
            f.op("dve", lambda e: e.tensor_scalar(out=posi[:, 2, :], in0=posi[:, 0, :], scalar1=icst[:, 4:5],
                                                  scalar2=None, op0=ALU.bitwise_and), reads=SMALL + [CON], writes=SMALL)
            f.op("dve", lambda e: e.tensor_copy(out=posf, in_=posi[:, 1:3, :]), reads=SMALL, writes=SMALL)
            I4 = idxf.rearrange("p (h q k) -> p h q k", h=8, q=2)
            O4x = Sx.rearrange("p (h k a) -> p h k a", h=8, k=16)
            O4y = Sy.rearrange("p (h k a) -> p h k a", h=8, k=16)
            for q in range(2):
                pf = posf[:, q, :].rearrange("p (h k) -> p h k", h=8)
                f.op("dve", lambda e, pf=pf: e.tensor_tensor(
                    out=O4y, in0=pf.unsqueeze(3).to_broadcast([128, 8, 16, 16]),
                    in1=iota16_f.unsqueeze(1).unsqueeze(1).to_broadcast([128, 8, 16, 16]), op=ALU.is_equal),
                    reads=SMALL + [CON], writes=SyB, n=2048)
                f.op("dve", lambda e, q=q: e.tensor_tensor(
                    out=O4x, in0=O4y, in1=I4[:, :, q, :].unsqueeze(2).to_broadcast([128, 8, 16, 16]), op=ALU.mult),
                    reads=SyB + SMALL, writes=SxB, n=2048)
                f.op("dve", lambda e, q=q: e.tensor_reduce(out=smis[:, s, q, :], in_=Sx.rearrange("p (j a) -> p j a", a=16),
                                                           axis=AX.X, op=ALU.add), reads=SxB, writes=[bf("smis")], n=2048)

    def wgen(blk):
        PAR = [bf("G1"), bf("G2"), bf("sA"), bf("sB"), bf("sC")]
        CH = [bf("Loh0"), bf("Loh1"), bf("R1oh0"), bf("R1oh1"), bf("Roh0"), bf("Roh1"), bf("wsm")]
        for nm in ("Loh0", "Loh1", "Roh0", "Roh1"):
            bf(nm).waw_ok = True
        f.op("pool", lambda e: e.memset(dummy[:, 0:1], 0.0), writes=PAR + CH + [bf("dummy")])
        for s in range(NSUB):
            T3 = T16s[:, s, :].rearrange("p (h k) -> p h k", h=8)
            w0 = wsm[:, 0, :].rearrange("p (h k) -> p h k", h=8)
            w1 = wsm[:, 1, :].rearrange("p (h k) -> p h k", h=8)
            f.op("dve", lambda e: e.tensor_tensor(out=w0, in0=T3, in1=T3[:, :, 0:1].to_broadcast([128, 8, 16]), op=ALU.subtract),
                 reads=[bf("T16s")], writes=[bf("wsm")])
            f.op("act", lambda e: e.activation(out=wsm[:, 0, :], in_=wsm[:, 0, :], func=AF.Exp), reads=[bf("wsm")], writes=[bf("wsm")])
            f.op("dve", lambda e: e.tensor_reduce(out=wzs[:, 0:8], in_=w0, axis=AX.X, op=ALU.add), reads=[bf("wsm")], writes=[bf("wsm")])
            f.op("dve", lambda e: e.reciprocal(out=wzs[:, 8:16], in_=wzs[:, 0:8]), reads=[bf("wsm")], writes=[bf("wsm")])
            f.op("dve", lambda e: e.tensor_tensor(out=w1, in0=w0, in1=wzs[:, 8:16].unsqueeze(2).to_broadcast([128, 8, 16]), op=ALU.mult),
                 reads=[bf("wsm")], writes=[bf("wsm")])
            b = next_ps()
            pso = ps[:, b, 0:128]
            f.op("pe", lambda e, pso=pso: e.transpose(pso, wsm[:, 1, :], ident), reads=[bf("wsm"), bf("ident")], writes=[PSB[b]])
            f.op("act", lambda e, pso=pso: e.activation(out=idxT[:, 2, s * 128:(s + 1) * 128], in_=pso, func=AF.Copy),
                 reads=[PSB[b]], writes=[bf("idxT")])
            for qi in range(2):
                b = next_ps()
                pso = ps[:, b, 0:128]
                f.op("pe", lambda e, qi=qi, pso=pso: e.transpose(pso, smis[:, s, qi, :], ident),
                     reads=[bf("smis"), bf("ident")], writes=[PSB[b]])
                f.op("act", lambda e, qi=qi, pso=pso: e.activation(out=idxT[:, qi, s * 128:(s + 1) * 128], in_=pso, func=AF.Copy),
                     reads=[PSB[b]], writes=[bf("idxT")])
        NG = TB // 16
        for tg in range(NG):
            o = tg % 2
            t0 = tg * 16
            for ti in range(16):
                tcol = t0 + ti
                f.op("dve", lambda e, ti=ti, tcol=tcol: e.tensor_scalar(
                    out=Loh[o][:, ti, :], in0=iota_bf, scalar1=idxT[:, 0, tcol:tcol + 1], scalar2=None,
                    op0=ALU.is_equal), reads=[bf("idxT"), CON], writes=[bf("Loh%d" % o)], n=64)
                f.op("dve", lambda e, ti=ti, tcol=tcol: e.tensor_scalar(
                    out=Roh[o][:, ti, :], in0=iota_bf, scalar1=idxT[:, 1, tcol:tcol + 1],
                    scalar2=idxT[:, 2, tcol:tcol + 1], op0=ALU.is_equal, op1=ALU.mult),
                    reads=[bf("idxT"), CON], writes=[bf("Roh%d" % o)], n=64)
            for q4 in range(4):
                b = 6 + ((tg * 4 + q4) % 2)
                for tt in range(4):
                    ti = q4 * 4 + tt
                    f.op("pe", lambda e, ti=ti, tt=tt, b=b: e.matmul(
                        ps[:, b, tt * 128:(tt + 1) * 128], Roh[o][:, ti, :], Loh[o][:, ti, :], start=True, stop=True),
                        reads=[bf("Roh%d" % o), bf("Loh%d" % o)], writes=[PSB[b]], sig=(tt == 3))
                tq = t0 + q4 * 4
                f.op("act", lambda e, b=b, tq=tq: e.activation(
                    out=Wb[:, :, tq:tq + 4], in_=ps[:, b, :].rearrange("p (t i) -> p i t", t=4), func=AF.Copy),
                    reads=[PSB[b]], writes=[bf("Wb")], n=512)
        f.op("pool", lambda e: e.memset(dummy[:, 0:1], 0.0), writes=CH + PAR + [bf("dummy")])

    def dense(blk, hook):
        n2c, n2b = n2T[blk % 2], bf("n2T%d" % (blk % 2))
        NGRP = 64
        SKEW = 4
        for s in range(NSUB):
            for dh in range(2):
                b = s * 2 + dh
                for kk in range(4):
                    kc = dh * 4 + kk
                    f.op("pe", lambda e, b=b, kk=kk, kc=kc, s=s: e.matmul(
                        ps[:, b, kk * 128:(kk + 1) * 128], xbuf[:, kc, s * 128:(s + 1) * 128], ident,
                        start=(kk == 0), stop=False), reads=[bf("xbuf"), bf("ident")], writes=[PSB[b]],
                        sig=(s == NSUB - 1 and dh == 1 and kk == 3), n=512)

        def load_u(g):
            i = g % 3
            f.dma(uring[i], ut_bt[g].rearrange("p (k c) -> p k c", k=KC),
                  reads=[bf("ut_b")], writes=[bf("uring%d" % i)])

        def load_v(g):
            i = g % 3
            f.dma(vring[i], v_b[g * 256:(g + 1) * 256, :].rearrange("(c p) d -> p c d", p=128),
                  reads=[bf("v_b")], writes=[bf("vring%d" % i)])

        def a_mm(c):
            g, cl = c // 2, c % 2
            i = g % 3
            b = 4 + c % 2
            pso = ps[:, b, 0:TB]
            for k in range(KC):
                f.op("pe", lambda e, k=k: e.matmul(pso, uring[i][:, k, cl * 128:(cl + 1) * 128], n2c[:, k, :],
                                                   start=(k == 0), stop=(k == KC - 1)),
                     reads=[bf("uring%d" % i), n2b], writes=[PSB[b]], sig=(k == KC - 1))
            f.op("act", lambda e: e.activation(out=gA[c % 4], in_=pso, func=AF.Gelu_apprx_tanh),
                 reads=[PSB[b]], writes=[bf("gA%d" % (c % 4))])
            f.op("pool", lambda e: e.tensor_tensor(out=Mb[c % 6], in0=gA[c % 4], in1=Wb[:, c, :], op=ALU.mult),
                 reads=[bf("gA%d" % (c % 4)), bf("Wb")], writes=[bf("Mb%d" % (c % 6))], n=200)

        def v_mm(c):
            g, cl = c // 2, c % 2
            i = g % 3
            for s in range(NSUB):
                for dh in range(2):
                    b = s * 2 + dh
                    last = (c == 127)
                    f.op("pe", lambda e, s=s, dh=dh, b=b: e.matmul(
                        ps[:, b, :], Mb[c % 6][:, s * 128:(s + 1) * 128], vring[i][:, cl, dh * 512:(dh + 1) * 512],
                        start=False, stop=last), reads=[bf("Mb%d" % (c % 6)), bf("vring%d" % i)],
                        writes=[PSB[b]], sig=(last or (s == NSUB - 1 and dh == 1)), n=512)

        load_u(0)
        load_v(0)
        load_u(1)
        load_v(1)
        for c in range(128 + SKEW):
            if c < 128:
                a_mm(c)
            if c >= SKEW:
                v_mm(c - SKEW)
            if c % 2 == 1 and c // 2 + 2 < NGRP:
                load_u(c // 2 + 2)
            cv = c - SKEW
            if cv >= 0 and cv % 2 == 1 and cv // 2 + 2 < NGRP:
                load_v(cv // 2 + 2)
            hook()
        for s in range(NSUB):
            for dh in range(2):
                b = s * 2 + dh
                f.op("act", lambda e, b=b: e.activation(out=junk, in_=ps[:, b, :], func=AF.Square,
                                                        accum_out=ssf[:, b:b + 1]),
                     reads=[PSB[b]], writes=[bf("ssf"), bf("junk")])
        for s in range(NSUB):
            f.op("dve", lambda e, s=s: e.tensor_tensor(out=ssf[:, 4 + s:5 + s], in0=ssf[:, 2 * s:2 * s + 1],
                                                       in1=ssf[:, 2 * s + 1:2 * s + 2], op=ALU.add),
                 reads=[bf("ssf")], writes=[bf("ssf")])
        f.op("act", lambda e: e.activation(out=ssf[:, 4:6], in_=ssf[:, 4:6], func=AF.Ln, scale=1.0 / D, bias=EPS),
             reads=[bf("ssf")], writes=[bf("ssf")])
        f.op("act", lambda e: e.activation(out=ssf[:, 6:8], in_=ssf[:, 4:6], func=AF.Exp, scale=-0.5),
             reads=[bf("ssf")], writes=[bf("ssf")])
        for s in range(NSUB):
            for dh in range(2):
                b = s * 2 + dh
                f.op("dve", lambda e, b=b, dh=dh, s=s: e.scalar_tensor_tensor(
                    out=osb[:, dh * 512:(dh + 1) * 512], in0=ps[:, b, :], scalar=ssf[:, 6 + s:7 + s],
                    in1=gfin[:, dh * 512:(dh + 1) * 512], op0=ALU.mult, op1=ALU.mult),
                    reads=[PSB[b], bf("ssf"), bf("gfin")], writes=[bf("osb")])
            r0 = blk * TB + s * 128
            f.dma(out_d[r0:r0 + 128, :], osb, reads=[bf("osb")], writes=[bf("outd")])

    f.dma(kt_s[:, 0:1024], k1_b, reads=[bf("k1_b")], writes=[bf("kt_s")])
    f.dma(kt_s[:, 1024:2048], k2_b, reads=[bf("k2_b")], writes=[bf("kt_s")])
    mixer(0, cur)
    if stop == 2:
        return finish()
    select(0)
    if stop == 3:
        return finish()
    wgen(0)
    if stop == 4:
        return finish()
    for blk in range(NBLK):
        nxt = blk + 1
        if nxt < NBLK and stop > 10:
            ncur = 1 - cur

            def stage1(nxt=nxt, ncur=ncur):
                mixer(nxt, ncur)
                select(nxt)
            co = Co(stage1)
            co_box[0] = co
            def hook():
                lim = f.vt["pe"] + 8.0
                cnt = 0
                f.last_start = 0.0
                while cnt < 200 and f.last_start <= lim:
                    cnt += 1
                    f.last_start = 0.0
                    if not co.step():
                        break
                    lim = max(lim, 0.0)
            dense(blk, hook)
            while co.step():
                pass
            co_box[0] = None
            wgen(nxt)
            cur = ncur
        else:
            dense(blk, lambda: None)
            if nxt < NBLK:
                cur = 1 - cur
                mixer(nxt, cur)
                select(nxt)
                wgen(nxt)
        if stop == 5:
            return finish()

    f.wait_all("sp", [bf("outd")])
    return nc, f


def _cols(v, n):
    return np.ascontiguousarray(np.asarray(v, np.float32).reshape(n, 128).T)


_CACHE = {}


def kernel(**inp):
    x = np.asarray(inp["x"], np.float32)
    Bsz, S, _ = x.shape
    n_cores = 8
    per = (Bsz * S) // n_cores
    halves = S // per
    assert halves == 2
    key = (per,)
    if key not in _CACHE:
        import os
        _CACHE[key] = build_program(NT=per, NPREV=per, stop=int(os.environ.get("KSTOP", "99")))
    nc, _ = _CACHE[key]

    params = np.zeros((128, NPAR), np.float32)
    params[:, P_NMIX:P_NMIX + 8] = _cols(inp["norm_mix"][0], 8)
    caw = np.asarray(inp["conv_a_w"][0], np.float32)
    for j in range(3):
        params[:, P_CAW + j * 8:P_CAW + (j + 1) * 8] = _cols(caw[j], 8)
    rcw = np.asarray(inp["rnn_conv_w"][0], np.float32)
    for j in range(4):
        params[:, P_RCW + j * 10:P_RCW + (j + 1) * 10] = _cols(rcw[j], 10)
    params[:, P_RCB:P_RCB + 10] = _cols(inp["rnn_conv_b"][0], 10)
    params[:, P_BRG:P_BRG + 10] = _cols(inp["b_rg"][0], 10)
    params[:, P_BIG:P_BIG + 10] = _cols(inp["b_ig"][0], 10)
    params[:, P_LAM:P_LAM + 10] = _cols(inp["lru_lambda"][0], 10)
    params[:, P_NFFN:P_NFFN + 8] = _cols(inp["norm_ffn"][0], 8)
    gfin = np.ascontiguousarray(np.broadcast_to(np.asarray(inp["norm_final"], np.float32)[None, :], (128, D)))
    ident = np.eye(128, dtype=np.float32)
    shared = {
        "gfin": gfin, "ident": ident,
        "w_in": np.ascontiguousarray(inp["w_in"][0], dtype=np.float32),
        "w_out_a": np.ascontiguousarray(inp["w_out_a"][0], dtype=np.float32),
        "w_out_b": np.ascontiguousarray(inp["w_out_b"][0], dtype=np.float32),
        "w_o": np.ascontiguousarray(inp["w_o"][0], dtype=np.float32),
        "w_q": np.ascontiguousarray(inp["w_q"][0], dtype=np.float32),
        "UT": np.ascontiguousarray(np.asarray(inp["expert_u"][0], np.float32).T),
        "V": np.ascontiguousarray(inp["expert_v"][0], dtype=np.float32),
        "K1T": np.ascontiguousarray(np.asarray(inp["sub_keys_1"][0], np.float32).transpose(2, 0, 1).reshape(128, 1024)),
        "K2T": np.ascontiguousarray(np.asarray(inp["sub_keys_2"][0], np.float32).transpose(2, 0, 1).reshape(128, 1024)),
        "wrg": np.ascontiguousarray(np.asarray(inp["w_rg"][0], np.float32).transpose(1, 0, 2).reshape(128, 1280)),
        "wig": np.ascontiguousarray(np.asarray(inp["w_ig"][0], np.float32).transpose(1, 0, 2).reshape(128, 1280)),
    }
    in_maps = []
    for c in range(n_cores):
        b, h = c // halves, c % halves
        xo = x[b, h * per:(h + 1) * per, :]
        m = dict(shared)
        m["xT"] = np.ascontiguousarray(xo.T)
        if h == 0:
            m["xpT"] = np.zeros((D, per), np.float32)
        else:
            m["xpT"] = np.ascontiguousarray(x[b, (h - 1) * per:h * per, :].T)
        p = params.copy()
        p[:, P_FLAG] = float(h)
        m["params"] = p
        in_maps.append(m)
    res = run_bass_kernel_spmd(nc, in_maps, core_ids=list(range(n_cores)))
    out = np.empty((Bsz, S, D), np.float32)
    for c in range(n_cores):
        b, h = c // halves, c % halves
        out[b, h * per:(h + 1) * per, :] = res.results[c]["out"]
    return out
```
